# Optimizing a Trainium2 kernel written in Bass

```python
import math
import jax
import jax.numpy as jnp
from jax import lax
import numpy as np

D_MODEL = 2048
BATCH = 2
SEQ = 4096
DEPTH = 2

GRID_W = 64
CTX_LEN = 256
EPS = 1e-6
ROPE_THETA = 10000.0

A_HEADS = 4
A_DK = 64
A_DV = 128
A_WIDTH = A_HEADS * A_DV
A_QK_COLS = A_HEADS * 2 * A_DK
Q_BLOCK = 128

B_WIDTH = 512

C_HEADS = 8
C_KV_HEADS = 2
C_GROUP = C_HEADS // C_KV_HEADS
C_DH = 128
C_WIDTH = C_HEADS * C_DH
C_KV_COLS = C_KV_HEADS * C_DH
WINDOW = 128
BAND_BLOCK = 128

MIX_WIDTH = A_WIDTH + B_WIDTH + C_WIDTH
GROUP_COLS = (A_QK_COLS, A_QK_COLS, A_WIDTH, B_WIDTH, B_WIDTH, B_WIDTH, C_WIDTH, C_KV_COLS, C_KV_COLS)
IN_COLS = sum(GROUP_COLS)
SPLIT_AT = tuple(int(v) for v in np.cumsum(GROUP_COLS)[:-1])

D_FF = 5632
N_EXPERTS = 8
TOP_K = 2
D_FF_EXPERT = 7168
N_DENSE = (DEPTH + 1) // 2
N_MOE = DEPTH // 2

kernel_name = 'hybrid_parallel_heads_diffusion_block'


def rms_norm(x, g):
    xf = x.astype(jnp.float32)
    y = xf * lax.rsqrt(jnp.mean(xf * xf, axis=-1, keepdims=True) + EPS)
    return (y * g.astype(jnp.float32)).astype(x.dtype)


def axial_rope_tables(n_tok, dh):
    rows = n_tok // GRID_W
    row = jnp.repeat(jnp.arange(rows, dtype=jnp.float32), GRID_W)
    col = jnp.tile(jnp.arange(GRID_W, dtype=jnp.float32), rows)
    half = dh // 2
    inv = ROPE_THETA ** (-jnp.arange(0, half, 2, dtype=jnp.float32) / half)
    ar = row[:, None] * inv[None, :]
    ac = col[:, None] * inv[None, :]
    ang = jnp.concatenate([ar, ar, ac, ac], axis=-1)
    return jnp.cos(ang), jnp.sin(ang)


def apply_rope(x, cos, sin):
    x1, x2, x3, x4 = jnp.split(x, 4, axis=-1)
    rot = jnp.concatenate([-x2, x1, -x4, x3], axis=-1)
    bshape = (x.shape[1],) + (1,) * (x.ndim - 3) + (x.shape[-1],)
    out = x.astype(jnp.float32) * cos.reshape(bshape) + rot.astype(jnp.float32) * sin.reshape(bshape)
    return out.astype(x.dtype)


def split_heads(p, qn_a, kn_a, qn_c, kn_c):
    b, n = p.shape[0], p.shape[1]
    a_q, a_k, a_v, b_b, b_c, b_x, c_q, c_k, c_v = jnp.split(p, SPLIT_AT, axis=-1)
    a_q = rms_norm(a_q.reshape(b, n, A_HEADS, 2, A_DK), qn_a)
    a_k = rms_norm(a_k.reshape(b, n, A_HEADS, 2, A_DK), kn_a)
    a_v = a_v.reshape(b, n, A_HEADS, A_DV)
    c_q = rms_norm(c_q.reshape(b, n, C_KV_HEADS, C_GROUP, C_DH), qn_c)
    c_k = rms_norm(c_k.reshape(b, n, C_KV_HEADS, C_DH), kn_c)
    c_v = c_v.reshape(b, n, C_KV_HEADS, C_DH)
    return a_q, a_k, a_v, b_b, b_c, b_x, c_q, c_k, c_v


def diff_lambda(lq1, lk1, lq2, lk2, lam_init):
    f = lambda u, v: jnp.exp(jnp.sum(u.astype(jnp.float32) * v.astype(jnp.float32)))
    return f(lq1, lk1) - f(lq2, lk2) + lam_init


def diff_attend(q, keys, vals, lam):
    s = jnp.einsum('bqhcd,bkhcd->bhcqk', q, keys, preferred_element_type=jnp.float32) * (A_DK ** -0.5)
    p = jax.nn.softmax(s, axis=-1)
    w = p[:, :, 0] - lam * p[:, :, 1]
    return jnp.einsum('bhqk,bkhd->bqhd', w.astype(vals.dtype), vals)


def diff_attention_latent(q, keys, vals, lam):
    b, s = q.shape[0], q.shape[1]
    nb = s // Q_BLOCK
    qb = jnp.moveaxis(q.reshape((b, nb, Q_BLOCK) + q.shape[2:]), 1, 0)
    out = lax.map(lambda qi: diff_attend(qi, keys, vals, lam), qb)
    return jnp.moveaxis(out, 0, 1).reshape(b, s, A_HEADS, A_DV)


def short_conv3(u, w):
    up = jnp.pad(u, ((0, 0), (1, 1), (0, 0)))
    return up[:, :-2] * w[0] + up[:, 1:-1] * w[1] + up[:, 2:] * w[2]


def banded(t):
    b, s = t.shape[0], t.shape[1]
    nb = s // BAND_BLOCK
    tb = t.reshape((b, nb, BAND_BLOCK) + t.shape[2:])
    tp = jnp.pad(tb, ((0, 0), (1, 1), (0, 0), (0, 0), (0, 0)))
    return jnp.concatenate([tp[:, :-2], tp[:, 1:-1], tp[:, 2:]], axis=2)


def band_mask(nb):
    a = jnp.arange(BAND_BLOCK)[:, None]
    j = jnp.arange(3 * BAND_BLOCK)[None, :]
    in_win = jnp.abs(j - a - BAND_BLOCK) <= WINDOW
    blk = jnp.arange(nb)[:, None] - 1 + (jnp.arange(3 * BAND_BLOCK) // BAND_BLOCK)[None, :]
    valid = (blk >= 0) & (blk < nb)
    return in_win[None] & valid[:, None, :]


def softmax_with_sink(s, sink_b):
    sk = jnp.broadcast_to(sink_b, s.shape[:-1] + (1,))
    return jax.nn.softmax(jnp.concatenate([s, sk], axis=-1), axis=-1)[..., :-1]


def window_attention_latent(q, k, v, k_ctx, v_ctx, sink):
    b, s = q.shape[0], q.shape[1]
    nb = s // BAND_BLOCK
    scale = C_DH ** -0.5
    qb = q.reshape(b, nb, BAND_BLOCK, C_KV_HEADS, C_GROUP, C_DH)
    kb, vb = banded(k), banded(v)
    s_loc = jnp.einsum('bnqkgd,bnjkd->bnkgqj', qb, kb, preferred_element_type=jnp.float32) * scale
    s_loc = jnp.where(band_mask(nb)[None, :, None, None], s_loc, -jnp.inf)
    s_ctx = jnp.einsum('bnqkgd,bjkd->bnkgqj', qb, k_ctx, preferred_element_type=jnp.float32) * scale
    sink_b = sink.astype(jnp.float32).reshape(1, 1, C_KV_HEADS, C_GROUP, 1, 1)
    p = softmax_with_sink(jnp.concatenate([s_loc, s_ctx], axis=-1), sink_b)
    n_loc = 3 * BAND_BLOCK
    o = (jnp.einsum('bnkgqj,bnjkd->bnqkgd', p[..., :n_loc].astype(v.dtype), vb)
         + jnp.einsum('bnkgqj,bjkd->bnqkgd', p[..., n_loc:].astype(v.dtype), v_ctx))
    return o.reshape(b, s, C_WIDTH)


def window_attention_context(q, k, v, sink):
    b, n = q.shape[0], q.shape[1]
    s = jnp.einsum('bqkgd,bjkd->bkgqj', q, k, preferred_element_type=jnp.float32) * (C_DH ** -0.5)
    p = softmax_with_sink(s, sink.astype(jnp.float32).reshape(1, C_KV_HEADS, C_GROUP, 1, 1))
    o = jnp.einsum('bkgqj,bjkd->bqkgd', p.astype(v.dtype), v)
    return o.reshape(b, n, C_WIDTH)


def swiglu(h, w1, w3, w2):
    return (jax.nn.silu(h @ w1) * (h @ w3)) @ w2


def moe_swiglu(h, w_r, w1, w3, w2):
    logits = (h @ w_r).astype(jnp.float32)
    top_v, top_i = lax.top_k(logits, TOP_K)
    gates = jax.nn.softmax(top_v, axis=-1)
    weight = jnp.sum(jax.nn.one_hot(top_i, N_EXPERTS, dtype=jnp.float32) * gates[..., None], axis=-2)
    y = jnp.zeros_like(h)
    for e in range(N_EXPERTS):
        y = y + weight[..., e:e + 1].astype(h.dtype) * swiglu(h, w1[e], w3[e], w2[e])
    return y


def setup_inputs(seed: int = 0) -> dict:
    key = jax.random.key(seed)
    ks = iter(jax.random.split(key, 32))
    f32 = jnp.float32
    D = D_MODEL

    def nrm(shape, scale):
        return jax.random.normal(next(ks), shape, f32) * scale

    def gain(shape):
        return 1.0 + 0.02 * jax.random.normal(next(ks), shape, f32)

    return {
        'x': nrm((BATCH, SEQ, D), 1.0),
        'c': nrm((BATCH, D), 1.0),
        'ctx': nrm((BATCH, CTX_LEN, D), 1.0),
        'c_ctx': nrm((D,), 1.0),
        'ada_w': nrm((DEPTH, D, 6 * D), 0.5 * D ** -0.5),
        'ada_b': nrm((DEPTH, 6 * D), 0.02),
        'norm_mix_g': gain((DEPTH, D)),
        'norm_ffn_g': gain((DEPTH, D)),
        'w_in': nrm((DEPTH, D, IN_COLS), D ** -0.5),
        'w_out': nrm((DEPTH, MIX_WIDTH, D), MIX_WIDTH ** -0.5),
        'a_q_norm': gain((DEPTH, A_DK)),
        'a_k_norm': gain((DEPTH, A_DK)),
        'a_lam_q1': nrm((DEPTH, A_DK), 0.1),
        'a_lam_k1': nrm((DEPTH, A_DK), 0.1),
        'a_lam_q2': nrm((DEPTH, A_DK), 0.1),
        'a_lam_k2': nrm((DEPTH, A_DK), 0.1),
        'a_subln_g': gain((DEPTH, A_DV)),
        'b_conv_w': nrm((DEPTH, 3, B_WIDTH), 3 ** -0.5),
        'b_out_g': gain((DEPTH, B_WIDTH)),
        'c_q_norm': gain((DEPTH, C_DH)),
        'c_k_norm': gain((DEPTH, C_DH)),
        'c_sink': nrm((DEPTH, C_HEADS), 0.5),
        'c_out_g': gain((DEPTH, C_WIDTH)),
        'ffn_w1': nrm((N_DENSE, D, D_FF), D ** -0.5),
        'ffn_w3': nrm((N_DENSE, D, D_FF), D ** -0.5),
        'ffn_w2': nrm((N_DENSE, D_FF, D), D_FF ** -0.5),
        'router_w': nrm((N_MOE, D, N_EXPERTS), D ** -0.5),
        'moe_w1': nrm((N_MOE, N_EXPERTS, D, D_FF_EXPERT), D ** -0.5),
        'moe_w3': nrm((N_MOE, N_EXPERTS, D, D_FF_EXPERT), D ** -0.5),
        'moe_w2': nrm((N_MOE, N_EXPERTS, D_FF_EXPERT, D), D_FF_EXPERT ** -0.5),
    }


def reference(x, c, ctx, c_ctx, ada_w, ada_b, norm_mix_g, norm_ffn_g, w_in, w_out,
              a_q_norm, a_k_norm, a_lam_q1, a_lam_k1, a_lam_q2, a_lam_k2, a_subln_g,
              b_conv_w, b_out_g, c_q_norm, c_k_norm, c_sink, c_out_g,
              ffn_w1, ffn_w3, ffn_w2, router_w, moe_w1, moe_w3, moe_w2):
    B, S, _ = x.shape
    L = ctx.shape[1]
    cos_a, sin_a = axial_rope_tables(S, A_DK)
    cos_c, sin_c = axial_rope_tables(S, C_DH)
    xc = ctx
    for l in range(DEPTH):
        last = l == DEPTH - 1
        mod = jax.nn.silu(c) @ ada_w[l] + ada_b[l]
        mod_c = jax.nn.silu(c_ctx) @ ada_w[l] + ada_b[l]
        sh_a, sc_a, g_a, sh_f, sc_f, g_f = [m[:, None, :] for m in jnp.split(mod, 6, axis=-1)]
        csh_a, csc_a, cg_a, csh_f, csc_f, cg_f = jnp.split(mod_c, 6, axis=-1)

        h = rms_norm(x, norm_mix_g[l]) * (1 + sc_a) + sh_a
        hc = rms_norm(xc, norm_mix_g[l]) * (1 + csc_a) + csh_a
        proj = jnp.concatenate([hc, h], axis=1) @ w_in[l]
        norms = (a_q_norm[l], a_k_norm[l], c_q_norm[l], c_k_norm[l])
        aq, ak, av, bb, bc, bx, cq, ck, cv = split_heads(proj[:, L:], *norms)
        aqc, akc, avc, bbc, bcc, bxc, cqc, ckc, cvc = split_heads(proj[:, :L], *norms)
        aq, ak = apply_rope(aq, cos_a, sin_a), apply_rope(ak, cos_a, sin_a)
        cq, ck = apply_rope(cq, cos_c, sin_c), apply_rope(ck, cos_c, sin_c)

        lam_init = 0.8 - 0.6 * math.exp(-0.3 * l)
        lam = diff_lambda(a_lam_q1[l], a_lam_k1[l], a_lam_q2[l], a_lam_k2[l], lam_init)

        ya = diff_attention_latent(aq, jnp.concatenate([ak, akc], axis=1),
                                   jnp.concatenate([av, avc], axis=1), lam)
        ya = (rms_norm(ya, a_subln_g[l]) * (1 - lam_init)).reshape(B, S, A_WIDTH)
        yb = rms_norm(bb * short_conv3(bc * bx, b_conv_w[l]), b_out_g[l])
        yc = rms_norm(window_attention_latent(cq, ck, cv, ckc, cvc, c_sink[l]), c_out_g[l])
        x = x + g_a * (jnp.concatenate([ya, yb, yc], axis=-1) @ w_out[l])

        if not last:
            ya_c = (rms_norm(diff_attend(aqc, akc, avc, lam), a_subln_g[l]) * (1 - lam_init)).reshape(B, L, A_WIDTH)
            yb_c = rms_norm(bbc * short_conv3(bcc * bxc, b_conv_w[l]), b_out_g[l])
            yc_c = rms_norm(window_attention_context(cqc, ckc, cvc, c_sink[l]), c_out_g[l])
            xc = xc + cg_a * (jnp.concatenate([ya_c, yb_c, yc_c], axis=-1) @ w_out[l])

        h2 = rms_norm(x, norm_ffn_g[l]) * (1 + sc_f) + sh_f
        if not last:
            h2c = rms_norm(xc, norm_ffn_g[l]) * (1 + csc_f) + csh_f
            tokens = jnp.concatenate([h2c, h2], axis=1)
        else:
            tokens = h2
        i = l // 2
        if l % 2 == 0:
            y = swiglu(tokens, ffn_w1[i], ffn_w3[i], ffn_w2[i])
        else:
            y = moe_swiglu(tokens, router_w[i], moe_w1[i], moe_w3[i], moe_w2[i])
        if not last:
            xc = xc + cg_f * y[:, :L]
            x = x + g_f * y[:, L:]
        else:
            x = x + g_f * y
    return x
```

```python
import contextlib
import math
import numpy as np
import ml_dtypes
import concourse.bass as bass
import concourse.mybir as mybir
from concourse.bass_utils import run_bass_kernel_spmd

F32 = mybir.dt.float32
BF16 = mybir.dt.bfloat16
ALU = mybir.AluOpType
AF = mybir.ActivationFunctionType
AX = mybir.AxisListType
COMPUTE = ("pe", "act", "dve", "pool")

D = 2048
KC = 16
SEQ = 4096
LCTX = 256
TOWN = 1024
NKS = 4480
NKB = NKS // 128
WOFF = 256
OWN0 = 384
EPS = 1e-6
D_FF = 5632
D_FFE = 7168
NEXP = 8
NEG = -30000.0


class T:
    __slots__ = ("name", "last_w", "readers", "dma_cnt")

    def __init__(self, name=""):
        self.name = name
        self.last_w = []
        self.readers = []
        self.dma_cnt = 0


class Op:
    __slots__ = ("eng", "fn", "deps", "sig", "is_dma", "sem", "val", "pos", "_barred", "_isbar")

    def __init__(self, eng, fn, is_dma):
        self.eng = eng
        self.fn = fn
        self.deps = []
        self.sig = False
        self.is_dma = is_dma
        self.sem = None
        self.val = None
        self._barred = False
        self._isbar = False


class Sched:
    def __init__(self, nc):
        self.nc = nc
        self.ops = {e: [] for e in ("pe", "act", "dve", "pool", "sp")}
        self.n_ops = 0

    def op(self, eng, fn, reads=(), writes=(), dma=False):
        o = Op(eng, fn, dma)
        o.pos = self.n_ops
        self.n_ops += 1
        deps = []
        for t in reads:
            deps.extend(t.last_w)
        for t in writes:
            deps.extend(t.last_w)
            deps.extend(t.readers)
        for t in reads:
            if not dma:
                t.readers = [r for r in t.readers if r.is_dma or r.eng != eng]
            t.readers.append(o)
        for t in writes:
            t.readers = []
        if dma:
            t = writes[0]
            t.dma_cnt += 1
            o.sem = ("dma", t)
            o.val = 16 * t.dma_cnt
            for t2 in writes:
                t2.last_w = [o]
        else:
            for t in writes:
                t.last_w = [o]
        best = {}
        rest = []
        seen = set()
        for d in deps:
            if d is o:
                continue
            if d.is_dma:
                if id(d) not in seen:
                    seen.add(id(d))
                    rest.append(d)
            else:
                b = best.get(d.eng)
                if b is None or d.pos > b.pos:
                    best[d.eng] = d
        o.deps = list(best.values()) + rest
        for d in o.deps:
            d.sig = True
        self.ops[eng].append(o)
        return o

    def barrier(self):
        lasts = []
        for e in COMPUTE:
            c = [o for o in self.ops[e] if not o.is_dma and not getattr(o, "_isbar", False)]
            if c:
                lasts.append(c[-1])
        dmas = [o for e in self.ops for o in self.ops[e] if o.is_dma and not o._barred]
        for e in self.ops:
            o = Op(e, lambda eng: None, False)
            o._isbar = True
            o.pos = self.n_ops
            self.n_ops += 1
            o.deps = [d for d in lasts if d.eng != e] + list(dmas)
            for d in o.deps:
                d.sig = True
            self.ops[e].append(o)
        for d in dmas:
            d._barred = True

    def emit(self, final_waits=()):
        nc = self.nc
        with contextlib.ExitStack() as es:
            sems = {e: es.enter_context(nc.semaphore("s_" + e)) for e in COMPUTE}
            tsems = {}
            for e in self.ops:
                for o in self.ops[e]:
                    if o.is_dma:
                        t = o.sem[1]
                        if id(t) not in tsems:
                            tsems[id(t)] = es.enter_context(nc.semaphore("d_%d" % len(tsems)))
            self.n_dma_sems = len(tsems)
            for e in COMPUTE:
                k = 0
                for o in self.ops[e]:
                    if o.sig and not o.is_dma:
                        k += 1
                        o.sem = sems[e]
                        o.val = k
            for e in self.ops:
                for o in self.ops[e]:
                    if o.is_dma:
                        o.sem = tsems[id(o.sem[1])]
            block = es.enter_context(nc.Block())

            def run(e, eng):
                waited = {}
                for o in self.ops[e]:
                    for d in o.deps:
                        key = id(d.sem)
                        if waited.get(key, 0) < d.val:
                            eng.wait_ge(d.sem, d.val)
                            waited[key] = d.val
                    inst = o.fn(eng)
                    if inst is None:
                        assert not o.sig and not o.is_dma
                        continue
                    if o.is_dma:
                        inst.then_inc(o.sem, 16)
                    elif o.sig:
                        inst.then_inc(o.sem, 1)
                if e == "sp":
                    for o in final_waits:
                        eng.wait_ge(o.sem, o.val)

            @block.tensor
            def _(eng):
                run("pe", eng)

            @block.scalar
            def _(eng):
                run("act", eng)

            @block.vector
            def _(eng):
                run("dve", eng)

            @block.gpsimd
            def _(eng):
                run("pool", eng)

            @block.sync
            def _(eng):
                run("sp", eng)


PP = {}
_off = 0
for _name, _n in (("cT", 32), ("ada_b", 96), ("nmix", 16), ("nffn", 16), ("aqn", 1), ("akn", 1),
                  ("cqn", 1), ("ckn", 1), ("lamv", 256), ("subln", 1), ("convw", 12), ("boutg", 4),
                  ("sink", 8), ("coutg", 8), ("routw", 128), ("hval", 2), ("kbias", NKB)):
    PP[_name] = (_off, _n)
    _off += _n
NPP = _off
CB_ONES, CB_BLK, CB_ROTA, CB_ROTC, CB_MLO, CB_MUP = [i * 128 for i in range(6)]
NCB = 6 * 128


class Builder:
    def __init__(self, nc):
        self.nc = nc
        self.S = Sched(nc)
        self.ps = [nc.alloc_psum_tensor("ps%d" % i, [128, 512], F32) for i in range(8)]
        self.psT = [T("ps%d" % i) for i in range(8)]
        self.uid = 0

    def sb(self, es, shape, dt, name=None):
        self.uid += 1
        return es.enter_context(self.nc.sbuf_tensor("%s_%d" % (name or "t", self.uid), list(shape), dt))

    def op(self, *a, **k):
        return self.S.op(*a, **k)

    def dma(self, q, out, in_, reads, wT, slow=False):
        ws = wT if isinstance(wT, (list, tuple)) else [wT]
        if slow:
            return self.S.op(q, lambda e: e.dma_start(out=out, in_=in_, allow_slow_non_contiguous=True), reads=reads, writes=list(ws), dma=True)
        return self.S.op(q, lambda e: e.dma_start(out=out, in_=in_), reads=reads, writes=list(ws), dma=True)

    def mm(self, bank, out_ap, pairs, reads):
        def fn(e):
            n = len(pairs)
            i = None
            for k, (l, r) in enumerate(pairs):
                i = e.matmul(out_ap, l, r, start=(k == 0), stop=(k == n - 1))
            return i
        return self.S.op("pe", fn, reads=reads, writes=[self.psT[bank]])

    def act(self, out, in_, func, reads, writes, scale=1.0, bias=0.0):
        return self.S.op("act", lambda e: e.activation(out=out, in_=in_, func=func, scale=scale, bias=bias),
                         reads=reads, writes=writes)

    def tt(self, out, in0, in1, op, reads, writes, eng="dve"):
        return self.S.op(eng, lambda e: e.tensor_tensor(out=out, in0=in0, in1=in1, op=op), reads=reads, writes=writes)

    def ts(self, out, in0, s1, s2, op0, op1, reads, writes, eng="dve"):
        if op1 is None:
            return self.S.op(eng, lambda e: e.tensor_scalar(out=out, in0=in0, scalar1=s1, scalar2=None, op0=op0),
                             reads=reads, writes=writes)
        return self.S.op(eng, lambda e: e.tensor_scalar(out=out, in0=in0, scalar1=s1, scalar2=s2, op0=op0, op1=op1),
                         reads=reads, writes=writes)

    def stt(self, out, in0, scalar, in1, op0, op1, reads, writes):
        return self.S.op("dve", lambda e: e.scalar_tensor_tensor(out=out, in0=in0, scalar=scalar, in1=in1, op0=op0, op1=op1),
                         reads=reads, writes=writes)

    def recip(self, out, in_, reads, writes):
        return self.S.op("dve", lambda e: e.reciprocal(out=out, in_=in_), reads=reads, writes=writes)

    def copy(self, out, in_, reads, writes, eng="dve"):
        if eng == "act":
            return self.act(out, in_, AF.Copy, reads, writes)
        return self.S.op(eng, lambda e: e.tensor_copy(out=out, in_=in_), reads=reads, writes=writes)


TS = 256


def flat(lst):
    out = []
    for x in lst:
        if isinstance(x, (list, tuple)):
            out.extend(flat(x))
        else:
            out.append(x)
    return out


class NormScratch:
    def __init__(self, B, P, n):
        self.n = n
        self.sq = [B.sb(P, [128, n], BF16, "nsq") for _ in range(3)]
        self.sqT = [T("nsq") for _ in range(3)]
        self.rs = B.sb(P, [128, n], F32, "nrs")
        self.rsT = T("nrs")
        self.tmp = [B.sb(P, [128, n], F32, "ntmp") for _ in range(3)]
        self.tmpT = [T("ntmp") for _ in range(3)]


class QKScratch:
    def __init__(self, B, P, n, nset=2):
        self.sets = []
        for _ in range(nset):
            self.sets.append(dict(
                sq=B.sb(P, [128, n], BF16, "qsq"), sqT=T("qsq"),
                rs=B.sb(P, [128, n], F32, "qrs"), rsT=T("qrs"),
                kg=B.sb(P, [128, n], BF16, "qkg"), kgT=T("qkg"),
                t1=B.sb(P, [128, n], F32, "qt1"), t1T=T("qt1"),
                t2=B.sb(P, [128, n], F32, "qt2"), t2T=T("qt2")))
        self.i = 0

    def next(self):
        s = self.sets[self.i % len(self.sets)]
        self.i += 1
        return s


def build_layer(B, lyr, dr, last):
    nc, S = B.nc, B.S
    ps, psT = B.ps, B.psT
    lam_init = 0.8 - 0.6 * math.exp(-0.3 * lyr)
    moe = (lyr % 2 == 1)
    own_tiles = ([] if last else [("ctx", 0, 256)]) + [("lat", OWN0 + i * TS, TS) for i in range(TOWN // TS)]

    with contextlib.ExitStack() as L:
        pp = B.sb(L, [128, NPP], F32, "pp")
        cb = B.sb(L, [128, NCB], BF16, "cb")
        idf = B.sb(L, [128, 128], F32, "idf")
        modT = B.sb(L, [128, 96, 2], F32, "modT")
        gsa = B.sb(L, [128, 16, 2], F32, "gsa")
        gsf = B.sb(L, [128, 16, 2], F32, "gsf")
        sml = B.sb(L, [128, 96], F32, "sml")
        epsc = B.sb(L, [128, 1], F32, "epsc")
        ppT, cbT, idT, modTT, gsT, smlT, epsT = T("pp"), T("cb"), T("idf"), T("mod"), T("gs"), T("sml"), T("eps")
        B.dma("sp", pp[:], dr["pp"], [], ppT)
        B.dma("sp", cb[:], dr["cb"], [], cbT)
        B.dma("sp", idf[:], dr["idf"], [], idT)
        S.op("dve", lambda e: e.memset(epsc[:], EPS), writes=[epsT])

        def ppc(name, i=0, n=1):
            o = PP[name][0] + i
            return pp[:, o:o + n]

        ones = cb[:, CB_ONES:CB_ONES + 128]
        blk64 = cb[:, CB_BLK:CB_BLK + 128]
        rotA = cb[:, CB_ROTA:CB_ROTA + 128]
        rotC = cb[:, CB_ROTC:CB_ROTC + 128]
        mlo = cb[:, CB_MLO:CB_MLO + 128]
        mup = cb[:, CB_MUP:CB_MUP + 128]

        with contextlib.ExitStack() as P:
            csil = B.sb(P, [128, 32], BF16, "csil")
            csT = T("csil")
            B.act(csil[:], ppc("cT", 0, 32), AF.Silu, [ppT], [csT])
            wsl = [B.sb(P, [128, KC, 256], BF16, "adaw") for _ in range(3)]
            wslT = [T("adaw%d" % i) for i in range(3)]
            MB = 7
            for g in range(48):
                s = g % 3
                B.dma("pool", wsl[s][:], dr["ada_w"][:, g * 256:(g + 1) * 256].rearrange("(kc p) f -> p kc f", p=128), [], wslT[s])
                for j in range(2):
                    n = g * 2 + j
                    B.mm(MB, ps[MB][:, 2 * n:2 * n + 2],
                         [(wsl[s][:, kc, j * 128:(j + 1) * 128], csil[:, 2 * kc:2 * kc + 2]) for kc in range(KC)],
                         [wslT[s], csT])
            for col in range(2):
                B.tt(modT[:, :, col], ps[MB][:, col:192:2], ppc("ada_b", 0, 96), ALU.add, [psT[MB], ppT], [modTT])
            for col in range(2):
                B.stt(gsa[:, :, col], modT[:, 16:32, col], 1.0, ppc("nmix", 0, 16), ALU.add, ALU.mult, [modTT, ppT], [gsT])
                B.stt(gsf[:, :, col], modT[:, 64:80, col], 1.0, ppc("nffn", 0, 16), ALU.add, ALU.mult, [modTT, ppT], [gsT])
            lv = PP["lamv"][0]
            S.op("dve", lambda e: e.tensor_tensor(out=sml[:, 32:96], in0=pp[:, lv:lv + 64], in1=pp[:, lv + 64:lv + 128], op=ALU.mult), reads=[ppT], writes=[smlT])
            S.op("dve", lambda e: e.tensor_reduce(out=sml[:, 1:2], in_=sml[:, 32:96], axis=AX.X, op=ALU.add), reads=[smlT], writes=[smlT])
            S.op("dve", lambda e: e.tensor_tensor(out=sml[:, 32:96], in0=pp[:, lv + 128:lv + 192], in1=pp[:, lv + 192:lv + 256], op=ALU.mult), reads=[ppT, smlT], writes=[smlT])
            S.op("dve", lambda e: e.tensor_reduce(out=sml[:, 2:3], in_=sml[:, 32:96], axis=AX.X, op=ALU.add), reads=[smlT], writes=[smlT])
            B.act(sml[:, 3:5], sml[:, 1:3], AF.Exp, [smlT], [smlT])
            B.tt(sml[:, 5:6], sml[:, 4:5], sml[:, 3:4], ALU.subtract, [smlT], [smlT])
            B.ts(sml[:, 0:1], sml[:, 5:6], -lam_init, None, ALU.add, None, [smlT], [smlT])
            B.ts(sml[:, 6:7], ppc("subln"), 1.0 - lam_init, None, ALU.mult, None, [ppT, smlT], [smlT])
            B.act(sml[:, 16:24], ppc("sink", 0, 8), AF.Exp, [ppT, smlT], [smlT])
        S.barrier()
        neg_lam = sml[:, 0:1]
        gsub = sml[:, 6:7]

        def mod_ap(which, kc, col):
            return modT[:, which * 16 + kc, col:col + 1]

        def norm_tile(sc, xs, xsT, n, hs, hT, gs, shw, col, bank, f32_cb=None):
            for kc in range(KC):
                k = kc % 3
                B.act(sc.sq[k][:, 0:n], xs[kc], AF.Square, flat(xsT[kc]), [sc.sqT[k]])
                S.op("pe", lambda e, kc=kc, k=k: e.matmul(ps[bank][:, 0:n], ones, sc.sq[k][:, 0:n], start=(kc == 0), stop=(kc == KC - 1)),
                     reads=[sc.sqT[k], cbT], writes=[psT[bank]])
            B.act(sc.rs[:, 0:n], ps[bank][:, 0:n], AF.Sqrt, [psT[bank], epsT], [sc.rsT], scale=1.0 / D, bias=epsc[:, 0:1])
            B.recip(sc.rs[:, 0:n], sc.rs[:, 0:n], [sc.rsT], [sc.rsT])
            for kc in range(KC):
                k = kc % 3
                B.tt(sc.tmp[k][:, 0:n], xs[kc], sc.rs[:, 0:n], ALU.mult, flat(xsT[kc]) + [sc.rsT], [sc.tmpT[k]])
                if f32_cb is None:
                    B.act(hs[kc], sc.tmp[k][:, 0:n], AF.Identity, [sc.tmpT[k], modTT, gsT], [hT],
                          scale=gs[:, kc, col:col + 1], bias=mod_ap(shw, kc, col))
                else:
                    f32_cb(kc, sc.tmp[k], sc.tmpT[k])

        def qk_finish(qs, bank, n, dh, gain_ap, rope, out_ap, outT, bank2):
            q = qs.next()
            B.act(q["sq"][:, 0:n], ps[bank][:, 0:n], AF.Square, [psT[bank]], [q["sqT"]])
            B.mm(bank2, ps[bank2][:, 0:n], [(blk64 if dh == 64 else ones, q["sq"][:, 0:n])], [q["sqT"], cbT])
            B.act(q["rs"][:, 0:n], ps[bank2][:, 0:n], AF.Sqrt, [psT[bank2], epsT], [q["rsT"]], scale=1.0 / dh, bias=epsc[:, 0:1])
            B.recip(q["rs"][:, 0:n], q["rs"][:, 0:n], [q["rsT"]], [q["rsT"]])
            if rope is None:
                B.stt(out_ap, ps[bank][:, 0:n], gain_ap, q["rs"][:, 0:n], ALU.mult, ALU.mult, [psT[bank], q["rsT"], ppT], [outT])
                return
            cos_ap, sin_ap, ropeT, rot = rope
            B.stt(q["kg"][:, 0:n], ps[bank][:, 0:n], gain_ap, q["rs"][:, 0:n], ALU.mult, ALU.mult, [psT[bank], q["rsT"], ppT], [q["kgT"]])
            B.mm(bank2, ps[bank2][:, 0:n], [(rot, q["kg"][:, 0:n])], [q["kgT"], cbT])
            B.tt(q["t1"][:, 0:n], q["kg"][:, 0:n], cos_ap, ALU.mult, [q["kgT"], ropeT], [q["t1T"]])
            B.tt(q["t2"][:, 0:n], ps[bank2][:, 0:n], sin_ap, ALU.mult, [psT[bank2], ropeT], [q["t2T"]])
            B.tt(out_ap, q["t1"][:, 0:n], q["t2"][:, 0:n], ALU.add, [q["t1T"], q["t2T"]], [outT], eng="pool")

        with contextlib.ExitStack() as P:
            wkv = B.sb(P, [128, KC, 1536], BF16, "wkv")
            wkvT = T("wkv")
            for (c_src, c_dst) in ((512, 0), (1024, 512), (4096, 1024)):
                B.dma("pool", wkv[:, :, c_dst:c_dst + 512],
                      dr["w_in"][:, c_src:c_src + 512].rearrange("(kc p) f -> p kc f", p=128), [], wkvT)
            KT = 512
            tiles = [(0, 256)] + [(c, min(KT, NKS - c)) for c in range(256, NKS, KT)]
            xb = [B.sb(P, [128, KC, KT], F32, "kvx") for _ in range(2)]
            xbT = [T("kvx0"), T("kvx1")]
            rp = [B.sb(P, [128, 4, KT], F32, "kvrope") for _ in range(2)]
            rpT = [T("rp0"), T("rp1")]
            hb = [B.sb(P, [128, KC, KT], BF16, "kvh") for _ in range(2)]
            hbT = [T("kvh0"), T("kvh1")]
            ko = [B.sb(P, [128, KT], BF16, "ko") for _ in range(3)]
            koT = [T("ko%d" % i) for i in range(3)]
            vo = [B.sb(P, [128, 768], BF16, "vo") for _ in range(2)]
            voT = [T("vo0"), T("vo1")]
            nsc = NormScratch(B, P, KT)
            qsc = QKScratch(B, P, KT, 2)
            kTA_T, kTC_T, vA_T, vC_T = T("kTA"), T("kTC"), T("vA"), T("vC")
            kcount = 0
            vcount = 0
            for ti, (c0, n) in enumerate(tiles):
                s = ti % 2
                col = 1 if c0 < 256 else 0
                need_c = c0 < 1536
                B.dma("sp", xb[s][:, :, 0:n], dr["xT"][:, c0:c0 + n].rearrange("(kc p) n -> p kc n", p=128), [], xbT[s])
                B.dma("sp", rp[s][:, :, 0:n], dr["rope"][:, :, c0:c0 + n], [], rpT[s])
                norm_tile(nsc, [xb[s][:, kc, 0:n] for kc in range(KC)], [[xbT[s]]] * KC, n,
                          [hb[s][:, kc, 0:n] for kc in range(KC)], hbT[s], gsa, 0, col, 0)
                chunks = [("A", h, h * 128) for h in range(4)] + ([("C", h, 1024 + h * 128) for h in range(2)] if need_c else [])
                for (grp, h, wc) in chunks:
                    bank = 1 + (kcount % 2)
                    k = kcount % 3
                    bank2 = 3 + (kcount % 2)
                    kcount += 1
                    B.mm(bank, ps[bank][:, 0:n], [(wkv[:, kc, wc:wc + 128], hb[s][:, kc, 0:n]) for kc in range(KC)], [wkvT, hbT[s]])
                    if grp == "A":
                        rope = (rp[s][:, 0, 0:n], rp[s][:, 1, 0:n], rpT[s], rotA)
                        qk_finish(qsc, bank, n, 64, ppc("akn"), rope, ko[k][:, 0:n], koT[k], bank2)
                        B.dma("sp", dr["kTA"][h, :, c0:c0 + n], ko[k][:, 0:n], [koT[k]], kTA_T)
                    else:
                        rope = (rp[s][:, 2, 0:n], rp[s][:, 3, 0:n], rpT[s], rotC)
                        qk_finish(qsc, bank, n, 128, ppc("ckn"), rope, ko[k][:, 0:n], koT[k], bank2)
                        B.dma("sp", dr["kTC"][h, :, c0:c0 + n], ko[k][:, 0:n], [koT[k]], kTC_T)
                for blk in range(n // 128):
                    vb = 5 + (vcount % 2)
                    vs = vcount % 2
                    vcount += 1
                    B.mm(vb, ps[vb][:, 0:512], [(hb[s][:, kc, blk * 128:(blk + 1) * 128], wkv[:, kc, 512:1024]) for kc in range(KC)], [wkvT, hbT[s]])
                    B.copy(vo[vs][:, 0:512], ps[vb][:, 0:512], [psT[vb]], [voT[vs]], eng="act")
                    r0 = c0 + blk * 128
                    B.dma("sp", dr["vA"][r0:r0 + 128, :], vo[vs][:, 0:512], [voT[vs]], vA_T)
                    if need_c:
                        B.mm(7, ps[7][:, 0:256], [(hb[s][:, kc, blk * 128:(blk + 1) * 128], wkv[:, kc, 1280:1536]) for kc in range(KC)], [wkvT, hbT[s]])
                        B.copy(vo[vs][:, 512:768], ps[7][:, 0:256], [psT[7]], [voT[vs]], eng="act")
                        B.dma("sp", dr["vC"][r0:r0 + 128, :], vo[vs][:, 512:768], [voT[vs]], vC_T)
        S.barrier()
        kTA_T, kTC_T, vA_T, vC_T = T("kTA"), T("kTC"), T("vA"), T("vC")

        NXT = TOWN // TS
        xown = B.sb(L, [128, KC, TOWN], F32, "xown")
        xownT = [[T("xo") for _ in range(NXT)] for _ in range(KC)]
        for kc in range(KC):
            B.dma("sp", xown[:, kc, :], dr["xT"][kc * 128:(kc + 1) * 128, OWN0:OWN0 + TOWN], [], xownT[kc])
        if not last:
            xc = B.sb(L, [128, KC, LCTX], F32, "xc")
            xcT = [T("xc") for _ in range(KC)]
            for kc in range(KC):
                B.dma("sp", xc[:, kc, :], dr["xT"][kc * 128:(kc + 1) * 128, 0:LCTX], [], xcT[kc])

        def xview(kind, c0, n):
            if kind == "ctx":
                return [xc[:, kc, :] for kc in range(KC)], [[xcT[kc]] for kc in range(KC)]
            o0 = c0 - OWN0
            return ([xown[:, kc, o0:o0 + n] for kc in range(KC)],
                    [[xownT[kc][i] for i in range(o0 // TS, (o0 + n) // TS)] for kc in range(KC)])

        with contextlib.ExitStack() as P:
            wg = [B.sb(P, [128, KC, 256], BF16, "wg") for _ in range(3)]
            wgT = [T("wg%d" % i) for i in range(3)]
            wg_i = [0]

            def load_w(src_ap):
                s = wg_i[0] % 3
                wg_i[0] += 1
                B.dma("pool", wg[s][:], src_ap, [], wgT[s])
                return wg[s], wgT[s]

            n = TS
            ht = B.sb(P, [128, KC, TS], BF16, "ht")
            htT = T("ht")
            hh = B.sb(P, [128, KC, 2], BF16, "hh")
            hhT = T("hh")
            hx = B.sb(P, [128, KC, 2], F32, "hx")
            hxT = T("hx")
            mix = B.sb(P, [128, 16, TS], BF16, "mix")
            mixT = [T("mix%d" % i) for i in range(16)]
            kA = B.sb(P, [128, NKS], BF16, "kA")
            kAT = T("kA")
            vAs = B.sb(P, [128, NKB, 128], BF16, "vAs")
            vAsT = T("vAs")
            NWB = TS // 128 + 2
            kCw = B.sb(P, [128, 2, NWB * 128], BF16, "kCw")
            kCc = B.sb(P, [128, 2, 256], BF16, "kCc")
            vCw = B.sb(P, [128, NWB, 256], BF16, "vCw")
            vCc = B.sb(P, [128, 2, 256], BF16, "vCc")
            kCwT, kCcT, vCwT, vCcT = T("kCw"), T("kCc"), T("vCw"), T("vCc")
            B.dma("sp", kCc[:], dr["kTC"][:, :, 0:256].rearrange("h p n -> p h n"), [kTC_T], kCcT)
            B.dma("sp", vCc[:], dr["vC"][0:256, :].rearrange("(kb p) d -> p kb d", p=128), [vC_T], vCcT)
            qa = B.sb(P, [128, 4, TS], BF16, "qa")
            qaT = [T("qa%d" % i) for i in range(4)]
            qc = B.sb(P, [128, 8, TS], BF16, "qc")
            qcT = [T("qc%d" % i) for i in range(8)]
            ropeq = B.sb(P, [128, 4, TS], F32, "ropeq")
            ropeqT = T("ropeq")
            U = B.sb(P, [128, TS + 2], F32, "U")
            UT = T("U")
            bxs = B.sb(P, [128, TS + 2], F32, "bxs")
            bxsT = T("bxs")
            ybp = B.sb(P, [128, 4, TS], F32, "ybp")
            ybpT = [T("ybp%d" % i) for i in range(4)]
            ycp = B.sb(P, [128, 8, TS], F32, "ycp")
            ycpT = [T("ycp%d" % i) for i in range(8)]
            pt = [B.sb(P, [128, 640], BF16, "pt") for _ in range(3)]
            ptT = [T("pt%d" % i) for i in range(3)]
            pt_i = [0]
            nsc = NormScratch(B, P, TS)
            nsc2 = NormScratch(B, P, 2)
            qsc = QKScratch(B, P, TS, 2)
            sqs = [B.sb(P, [128, TS], BF16, "sqs") for _ in range(2)]
            sqsT = [T("sqs0"), T("sqs1")]
            sq_i = [0]
            rsb = B.sb(P, [128, TS], F32, "rsb")
            rsbT = T("rsb")
            r0 = B.sb(P, [128, TS], F32, "r0")
            r1 = B.sb(P, [128, TS], F32, "r1")
            r0T, r1T = T("r0"), T("r1")
            rc = [B.sb(P, [128, 128], F32, "rc") for _ in range(2)]
            rcT = [T("rc0"), T("rc1")]
            rc_i = [0]

            def sumsq_chunks(bank, srcs, srcTs):
                m = len(srcs)
                for i in range(m):
                    k = sq_i[0] % 2
                    sq_i[0] += 1
                    B.act(sqs[k][:, 0:n], srcs[i], AF.Square, [srcTs[i]], [sqsT[k]])
                    S.op("pe", lambda e, i=i, k=k, n=n, m=m, bank=bank: e.matmul(ps[bank][:, 0:n], ones, sqs[k][:, 0:n], start=(i == 0), stop=(i == m - 1)),
                         reads=[sqsT[k], cbT], writes=[psT[bank]])

            for (kind, c0, _n) in own_tiles:
                col = 1 if kind == "ctx" else 0
                lat = kind == "lat"
                i_t = (c0 - OWN0) // TS
                xs, xsT = xview(kind, c0, n)
                norm_tile(nsc, xs, xsT, n, [ht[:, kc, 0:n] for kc in range(KC)], htT, gsa, 0, col, 0)
                if lat:
                    B.dma("sp", hx[:, :, 0:1], dr["xT"][:, c0 - 1:c0].rearrange("(kc p) n -> p kc n", p=128), [], hxT, slow=True)
                    B.dma("sp", hx[:, :, 1:2], dr["xT"][:, c0 + n:c0 + n + 1].rearrange("(kc p) n -> p kc n", p=128), [], hxT, slow=True)
                    norm_tile(nsc2, [hx[:, kc, :] for kc in range(KC)], [[hxT]] * KC, 2, [hh[:, kc, :] for kc in range(KC)], hhT, gsa, 0, 0, 0)
                    B.dma("sp", ropeq[:, :, 0:n], dr["rope"][:, :, c0:c0 + n], [], ropeqT)
                for g in range(2):
                    wbb, wbbT = load_w(dr["w_in"][:, 1536 + g * 256:1536 + (g + 1) * 256].rearrange("(kc p) f -> p kc f", p=128))
                    wbc, wbcT = load_w(dr["w_in"][:, 2048 + g * 256:2048 + (g + 1) * 256].rearrange("(kc p) f -> p kc f", p=128))
                    wbx, wbxT = load_w(dr["w_in"][:, 2560 + g * 256:2560 + (g + 1) * 256].rearrange("(kc p) f -> p kc f", p=128))
                    for j in range(2):
                        f = g * 2 + j
                        sl = slice(j * 128, (j + 1) * 128)
                        B.mm(1, ps[1][:, 0:n], [(wbc[:, kc, sl], ht[:, kc, 0:n]) for kc in range(KC)], [wbcT, htT])
                        B.mm(2, ps[2][:, 0:n], [(wbx[:, kc, sl], ht[:, kc, 0:n]) for kc in range(KC)], [wbxT, htT])
                        B.mm(3, ps[3][:, 0:n], [(wbb[:, kc, sl], ht[:, kc, 0:n]) for kc in range(KC)], [wbbT, htT])
                        B.copy(bxs[:, 1:n + 1], ps[2][:, 0:n], [psT[2]], [bxsT], eng="act")
                        B.tt(U[:, 1:n + 1], ps[1][:, 0:n], bxs[:, 1:n + 1], ALU.mult, [psT[1], bxsT], [UT])
                        if lat:
                            B.mm(1, ps[1][:, 0:2], [(wbc[:, kc, sl], hh[:, kc, :]) for kc in range(KC)], [wbcT, hhT])
                            B.mm(2, ps[2][:, 0:2], [(wbx[:, kc, sl], hh[:, kc, :]) for kc in range(KC)], [wbxT, hhT])
                            B.copy(bxs[:, 0:1], ps[2][:, 0:1], [psT[2]], [bxsT], eng="act")
                            B.copy(bxs[:, n + 1:n + 2], ps[2][:, 1:2], [psT[2]], [bxsT], eng="act")
                            B.tt(U[:, 0:1], ps[1][:, 0:1], bxs[:, 0:1], ALU.mult, [psT[1], bxsT], [UT])
                            B.tt(U[:, n + 1:n + 2], ps[1][:, 1:2], bxs[:, n + 1:n + 2], ALU.mult, [psT[1], bxsT], [UT])
                            if i_t == 0:
                                B.ts(U[:, 0:1], U[:, 0:1], ppc("hval", 0), None, ALU.mult, None, [UT, ppT], [UT])
                            if i_t == NXT - 1:
                                B.ts(U[:, n + 1:n + 2], U[:, n + 1:n + 2], ppc("hval", 1), None, ALU.mult, None, [UT, ppT], [UT])
                        else:
                            S.op("dve", lambda e: e.memset(U[:, 0:1], 0.0), writes=[UT])
                            S.op("dve", lambda e, n=n: e.memset(U[:, n + 1:n + 2], 0.0), writes=[UT])
                        cw = PP["convw"][0] + f * 3
                        B.ts(ybp[:, f, 0:n], U[:, 0:n], pp[:, cw:cw + 1], None, ALU.mult, None, [UT, ppT], [ybpT[f]])
                        B.stt(ybp[:, f, 0:n], U[:, 1:n + 1], pp[:, cw + 1:cw + 2], ybp[:, f, 0:n], ALU.mult, ALU.add, [UT, ppT, ybpT[f]], [ybpT[f]])
                        B.stt(ybp[:, f, 0:n], U[:, 2:n + 2], pp[:, cw + 2:cw + 3], ybp[:, f, 0:n], ALU.mult, ALU.add, [UT, ppT, ybpT[f]], [ybpT[f]])
                        B.tt(ybp[:, f, 0:n], ybp[:, f, 0:n], ps[3][:, 0:n], ALU.mult, [ybpT[f], psT[3]], [ybpT[f]])
                sumsq_chunks(4, [ybp[:, f, 0:n] for f in range(4)], ybpT)
                B.act(rsb[:, 0:n], ps[4][:, 0:n], AF.Sqrt, [psT[4], epsT], [rsbT], scale=1.0 / 512, bias=epsc[:, 0:1])
                B.recip(rsb[:, 0:n], rsb[:, 0:n], [rsbT], [rsbT])
                for f in range(4):
                    B.stt(mix[:, 4 + f, 0:n], ybp[:, f, 0:n], ppc("boutg", f), rsb[:, 0:n], ALU.mult, ALU.mult, [ybpT[f], rsbT, ppT], [mixT[4 + f]])

                for g in range(2):
                    wq, wqT = load_w(dr["w_in"][:, g * 256:(g + 1) * 256].rearrange("(kc p) f -> p kc f", p=128))
                    for j in range(2):
                        h = g * 2 + j
                        bank = 1 + (h % 2)
                        B.mm(bank, ps[bank][:, 0:n], [(wq[:, kc, j * 128:(j + 1) * 128], ht[:, kc, 0:n]) for kc in range(KC)], [wqT, htT])
                        rope = (ropeq[:, 0, 0:n], ropeq[:, 1, 0:n], ropeqT, rotA) if lat else None
                        qk_finish(qsc, bank, n, 64, ppc("aqn"), rope, qa[:, h, 0:n], qaT[h], 3)
                nkb = NKB if lat else 2
                for h in range(4):
                    B.dma("sp", kA[:, 0:nkb * 128], dr["kTA"][h, :, 0:nkb * 128], [kTA_T], kAT)
                    B.dma("sp", vAs[:, 0:nkb, :], dr["vA"][0:nkb * 128, h * 128:(h + 1) * 128].rearrange("(kb p) d -> p kb d", p=128), [vA_T], vAsT)
                    for c in range(2):
                        ob, db = 4 + c, 6 + c
                        pending = []
                        for kb in range(nkb):
                            sbk = 1 + (kb % 2)
                            B.mm(sbk, ps[sbk][:, 0:n], [(kA[c * 64:(c + 1) * 64, kb * 128:(kb + 1) * 128], qa[c * 64:(c + 1) * 64, h, 0:n])], [kAT, qaT[h]])
                            k = pt_i[0] % 3
                            pt_i[0] += 1
                            kbo = PP["kbias"][0] + kb
                            B.act(pt[k][:, 0:n], ps[sbk][:, 0:n], AF.Exp, [psT[sbk], ppT], [ptT[k]], scale=0.125, bias=pp[:, kbo:kbo + 1])
                            pending.append((k, kb))
                            if len(pending) == 2:
                                kk, kb2 = pending.pop(0)
                                _pv(B, ps, psT, ob, db, n, vAs, vAsT, pt, ptT, ones, cbT, kk, kb2, nkb)
                        while pending:
                            kk, kb2 = pending.pop(0)
                            _pv(B, ps, psT, ob, db, n, vAs, vAsT, pt, ptT, ones, cbT, kk, kb2, nkb)
                    B.recip(r0[:, 0:n], ps[6][:, 0:n], [psT[6]], [r0T])
                    B.recip(r1[:, 0:n], ps[7][:, 0:n], [psT[7]], [r1T])
                    B.tt(r0[:, 0:n], ps[4][:, 0:n], r0[:, 0:n], ALU.mult, [psT[4], r0T], [r0T])
                    B.tt(r1[:, 0:n], ps[5][:, 0:n], r1[:, 0:n], ALU.mult, [psT[5], r1T], [r1T])
                    B.stt(r0[:, 0:n], r1[:, 0:n], neg_lam, r0[:, 0:n], ALU.mult, ALU.add, [r0T, r1T, smlT], [r0T])
                    sumsq_chunks(3, [r0[:, 0:n]], [r0T])
                    B.act(r1[:, 0:n], ps[3][:, 0:n], AF.Sqrt, [psT[3], epsT, r1T], [r1T], scale=1.0 / 128, bias=epsc[:, 0:1])
                    B.recip(r1[:, 0:n], r1[:, 0:n], [r1T], [r1T])
                    B.stt(mix[:, h, 0:n], r0[:, 0:n], gsub, r1[:, 0:n], ALU.mult, ALU.mult, [r0T, r1T, smlT], [mixT[h]])

                if lat:
                    wc0 = c0 - 128
                    B.dma("sp", kCw[:], dr["kTC"][:, :, wc0:wc0 + NWB * 128].rearrange("h p n -> p h n"), [kTC_T], kCwT)
                    B.dma("sp", vCw[:], dr["vC"][wc0:wc0 + NWB * 128, :].rearrange("(kb p) d -> p kb d", p=128), [vC_T], vCwT)
                for g in range(4):
                    wq, wqT = load_w(dr["w_in"][:, 3072 + g * 256:3072 + (g + 1) * 256].rearrange("(kc p) f -> p kc f", p=128))
                    for j in range(2):
                        hd = g * 2 + j
                        bank = 1 + (hd % 2)
                        B.mm(bank, ps[bank][:, 0:n], [(wq[:, kc, j * 128:(j + 1) * 128], ht[:, kc, 0:n]) for kc in range(KC)], [wqT, htT])
                        rope = (ropeq[:, 2, 0:n], ropeq[:, 3, 0:n], ropeqT, rotC) if lat else None
                        qk_finish(qsc, bank, n, 128, ppc("cqn"), rope, qc[:, hd, 0:n], qcT[hd], 3)
                cscale = 128.0 ** -0.5
                for hd in range(8):
                    kvh = hd // 4
                    for qb in range(n // 128):
                        qsl = slice(qb * 128, (qb + 1) * 128)
                        q_ap = qc[:, hd, qsl]
                        k = pt_i[0] % 3
                        pt_i[0] += 1
                        blocks = []
                        if lat:
                            for w in range(3):
                                wb = qb + w
                                B.mm(1, ps[1][:, w * 128:(w + 1) * 128], [(kCw[:, kvh, wb * 128:(wb + 1) * 128], q_ap)], [kCwT, qcT[hd]])
                            for w in range(2):
                                B.mm(2, ps[2][:, w * 128:(w + 1) * 128], [(kCc[:, kvh, w * 128:(w + 1) * 128], q_ap)], [kCcT, qcT[hd]])
                            kb0 = (c0 - 128) // 128 + qb
                            for w in range(3):
                                kbo = PP["kbias"][0] + kb0 + w
                                B.act(pt[k][:, w * 128:(w + 1) * 128], ps[1][:, w * 128:(w + 1) * 128], AF.Exp, [psT[1], ppT], [ptT[k]],
                                      scale=cscale, bias=pp[:, kbo:kbo + 1])
                                blocks.append((pt[k][:, w * 128:(w + 1) * 128], vCw[:, qb + w, kvh * 128:(kvh + 1) * 128], vCwT))
                            B.act(pt[k][:, 384:640], ps[2][:, 0:256], AF.Exp, [psT[2]], [ptT[k]], scale=cscale)
                            B.tt(pt[k][:, 0:128], pt[k][:, 0:128], mlo, ALU.mult, [ptT[k], cbT], [ptT[k]])
                            B.tt(pt[k][:, 256:384], pt[k][:, 256:384], mup, ALU.mult, [ptT[k], cbT], [ptT[k]])
                            for w in range(2):
                                blocks.append((pt[k][:, 384 + w * 128:384 + (w + 1) * 128], vCc[:, w, kvh * 128:(kvh + 1) * 128], vCcT))
                        else:
                            for w in range(2):
                                B.mm(2, ps[2][:, w * 128:(w + 1) * 128], [(kCc[:, kvh, w * 128:(w + 1) * 128], q_ap)], [kCcT, qcT[hd]])
                            B.act(pt[k][:, 384:640], ps[2][:, 0:256], AF.Exp, [psT[2]], [ptT[k]], scale=cscale)
                            for w in range(2):
                                blocks.append((pt[k][:, 384 + w * 128:384 + (w + 1) * 128], vCc[:, w, kvh * 128:(kvh + 1) * 128], vCcT))
                        vts = list({id(b[2]): b[2] for b in blocks}.values())
                        B.mm(4, ps[4][:, 0:128], [(v_ap, p_ap) for (p_ap, v_ap, _) in blocks], [ptT[k]] + vts)
                        B.mm(5, ps[5][:, 0:128], [(ones, p_ap) for (p_ap, _, _) in blocks], [ptT[k], cbT])
                        rk = rc_i[0] % 2
                        rc_i[0] += 1
                        B.ts(rc[rk][:], ps[5][:, 0:128], sml[:, 16 + hd:17 + hd], None, ALU.add, None, [psT[5], smlT], [rcT[rk]])
                        B.recip(rc[rk][:], rc[rk][:], [rcT[rk]], [rcT[rk]])
                        B.tt(ycp[:, hd, qsl], ps[4][:, 0:128], rc[rk][:], ALU.mult, [psT[4], rcT[rk]], [ycpT[hd]])
                sumsq_chunks(3, [ycp[:, hd, 0:n] for hd in range(8)], ycpT)
                B.act(rsb[:, 0:n], ps[3][:, 0:n], AF.Sqrt, [psT[3], epsT, rsbT], [rsbT], scale=1.0 / 1024, bias=epsc[:, 0:1])
                B.recip(rsb[:, 0:n], rsb[:, 0:n], [rsbT], [rsbT])
                for hd in range(8):
                    B.stt(mix[:, 8 + hd, 0:n], ycp[:, hd, 0:n], ppc("coutg", hd), rsb[:, 0:n], ALU.mult, ALU.mult, [ycpT[hd], rsbT, ppT], [mixT[8 + hd]])

                for g in range(8):
                    wo, woT = load_w(dr["w_out"][:, g * 256:(g + 1) * 256].rearrange("(mc p) d -> p mc d", p=128))
                    for j in range(2):
                        dc = g * 2 + j
                        bank = 1 + (dc % 2)
                        B.mm(bank, ps[bank][:, 0:n], [(wo[:, mc, j * 128:(j + 1) * 128], mix[:, mc, 0:n]) for mc in range(16)], [woT] + mixT)
                        B.stt(xs[dc], ps[bank][:, 0:n], mod_ap(2, dc, col), xs[dc], ALU.mult, ALU.add, [psT[bank], modTT] + flat(xsT[dc]), flat(xsT[dc]))
        S.barrier()

        ffn_tiles = ([] if last else [("ctx", 0, 256)]) + [("lat", OWN0, 512), ("lat", OWN0 + 512, 512)]
        NT = sum(t[2] for t in ffn_tiles)
        toff = []
        o = 0
        for t in ffn_tiles:
            toff.append(o)
            o += t[2]
        with contextlib.ExitStack() as P:
            h2 = B.sb(P, [128, KC, NT], BF16, "h2")
            h2T = T("h2")
            moe_state = None
            if moe:
                wgt = B.sb(P, [128, 8, 8], F32, "wgt")
                wgtT = [T("wgt%d" % i) for i in range(8)]
                wT8 = B.sb(P, [8, TOWN], F32, "wT8")
                wT8T = T("wT8")
                sel = B.sb(P, [8, 1024], F32, "sel")
                selT = T("sel")
                B.dma("sp", sel[:], dr["sel"], [], selT)
                moe_state = (wgt, wgtT, wT8, wT8T, sel, selT, idf, idT)
            with contextlib.ExitStack() as P2:
                nsc = NormScratch(B, P2, 512)
                if moe:
                    wr = B.sb(P2, [128, KC, 8], F32, "wr")
                    wrT = T("wr")
                    ro = PP["routw"][0]
                    B.copy(wr[:].rearrange("p a b -> p (a b)"), pp[:, ro:ro + 128], [ppT], [wrT])
                    hf = [B.sb(P2, [128, 512], F32, "hf") for _ in range(3)]
                    hfT = [T("hf%d" % i) for i in range(3)]
                LB = 7
                for ti, (kind, c0, n) in enumerate(ffn_tiles):
                    col = 1 if kind == "ctx" else 0
                    xs, xsT = xview(kind, c0, n)
                    hs = [h2[:, kc, toff[ti]:toff[ti] + n] for kc in range(KC)]
                    if not moe:
                        norm_tile(nsc, xs, xsT, n, hs, h2T, gsf, 3, col, 0)
                    else:
                        def cb_(kc, tmp, tmpT_, hs=hs, ti=ti, n=n):
                            k = kc % 3
                            B.act(hf[k][:, 0:n], tmp[:, 0:n], AF.Identity, [tmpT_, modTT, gsT], [hfT[k]],
                                  scale=gsf[:, kc, 0:1], bias=mod_ap(3, kc, 0))
                            B.copy(hs[kc], hf[k][:, 0:n], [hfT[k]], [h2T], eng="pool")
                            for blk in range(n // 128):
                                gb = ti * 4 + blk
                                S.op("pe", lambda e, k=k, blk=blk, kc=kc, gb=gb: e.matmul(
                                    ps[LB][:, gb * 8:gb * 8 + 8], hf[k][:, blk * 128:(blk + 1) * 128], wr[:, kc, :],
                                    start=(kc == 0 and gb == 0), stop=(kc == KC - 1), skip_group_check=True),
                                     reads=[hfT[k], wrT], writes=[psT[LB]])
                        norm_tile(nsc, xs, xsT, n, hs, h2T, gsf, 3, col, 0, f32_cb=cb_)
                if moe:
                    lg = B.sb(P2, [128, 8, 8], F32, "lg")
                    lgT = T("lg")
                    B.copy(lg[:].rearrange("p a b -> p (a b)"), ps[LB][:, 0:64], [psT[LB]], [lgT])
                    m1 = B.sb(P2, [128, 8], F32, "m1")
                    m2 = B.sb(P2, [128, 8], F32, "m2")
                    eq1 = B.sb(P2, [128, 8, 8], F32, "eq1")
                    eq2 = B.sb(P2, [128, 8, 8], F32, "eq2")
                    l2 = B.sb(P2, [128, 8, 8], F32, "l2")
                    gg = B.sb(P2, [128, 8, 4], F32, "gg")
                    gT = T("gate")
                    for b8 in range(8):
                        S.op("dve", lambda e, b8=b8: e.tensor_reduce(out=m1[:, b8:b8 + 1], in_=lg[:, b8, :], axis=AX.X, op=ALU.max), reads=[lgT], writes=[gT])
                        B.ts(eq1[:, b8, :], lg[:, b8, :], m1[:, b8:b8 + 1], None, ALU.is_equal, None, [lgT, gT], [gT])
                        B.stt(l2[:, b8, :], eq1[:, b8, :], -1e30, lg[:, b8, :], ALU.mult, ALU.add, [gT, lgT], [gT])
                        S.op("dve", lambda e, b8=b8: e.tensor_reduce(out=m2[:, b8:b8 + 1], in_=l2[:, b8, :], axis=AX.X, op=ALU.max), reads=[gT], writes=[gT])
                        B.ts(eq2[:, b8, :], l2[:, b8, :], m2[:, b8:b8 + 1], None, ALU.is_equal, None, [gT], [gT])
                        B.tt(gg[:, b8, 0:1], m2[:, b8:b8 + 1], m1[:, b8:b8 + 1], ALU.subtract, [gT], [gT])
                        B.act(gg[:, b8, 1:2], gg[:, b8, 0:1], AF.Exp, [gT], [gT])
                        B.ts(gg[:, b8, 2:3], gg[:, b8, 1:2], 1.0, None, ALU.add, None, [gT], [gT])
                        B.recip(gg[:, b8, 2:3], gg[:, b8, 2:3], [gT], [gT])
                        B.tt(gg[:, b8, 3:4], gg[:, b8, 1:2], gg[:, b8, 2:3], ALU.mult, [gT], [gT])
                        B.ts(wgt[:, b8, :], eq1[:, b8, :], gg[:, b8, 2:3], None, ALU.mult, None, [gT], [wgtT[b8]])
                        B.stt(wgt[:, b8, :], eq2[:, b8, :], gg[:, b8, 3:4], wgt[:, b8, :], ALU.mult, ALU.add, [gT, wgtT[b8]], [wgtT[b8]])
            S.barrier()
            _ffn_main(B, P, dr, lyr, moe, ffn_tiles, toff, NT, h2, xview, mod_ap, modTT, moe_state)
        S.barrier()

        outs = []
        oT = T("out")
        for kc in range(KC):
            outs.append(B.dma("sp", dr["xo"][kc * 128:(kc + 1) * 128, :], xown[:, kc, :], flat(xownT[kc]), oT))
        if not last:
            ocT = T("outc")
            for kc in range(KC):
                outs.append(B.dma("sp", dr["xco"][kc * 128:(kc + 1) * 128, :], xc[:, kc, :], [xcT[kc]], ocT))
        S.barrier()
        return outs


def _pv(B, ps, psT, ob, db, n, vAs, vAsT, pt, ptT, ones, cbT, k, kb, nkb):
    B.S.op("pe", lambda e: e.matmul(ps[ob][:, 0:n], vAs[:, kb, :], pt[k][:, 0:n], start=(kb == 0), stop=(kb == nkb - 1)),
           reads=[vAsT, ptT[k]], writes=[psT[ob]])
    B.S.op("pe", lambda e: e.matmul(ps[db][:, 0:n], ones, pt[k][:, 0:n], start=(kb == 0), stop=(kb == nkb - 1)),
           reads=[cbT, ptT[k]], writes=[psT[db]])


def _ffn_main(B, P, dr, lyr, moe, ffn_tiles, toff, NT, h2, xview, mod_ap, modTT, moe_state):
    nc, S, ps, psT = B.nc, B.S, B.ps, B.psT
    h2T = T("h2all")
    G = 2
    NS = 2
    if moe:
        wgt, wgtT, wT8, wT8T, sel, selT, idf, idT = moe_state
        for b8 in range(8):
            bank = 6 if b8 < 4 else 5
            cb0 = (b8 % 4) * 128
            S.op("pe", lambda e, b8=b8, bank=bank, cb0=cb0: e.transpose(ps[bank][0:8, cb0:cb0 + 128], wgt[:, b8, :], idf[:, :]),
                 reads=[wgtT[b8], idT], writes=[psT[bank]])
        B.copy(wT8[:, 0:512], ps[6][0:8, 0:512], [psT[6]], [wT8T])
        B.copy(wT8[:, 512:1024], ps[5][0:8, 0:512], [psT[5]], [wT8T])
        wb = [B.sb(P, [128, TOWN], F32, "wb") for _ in range(2)]
        wbT = [T("wb0"), T("wb1")]
    nexp = NEXP if moe else 1
    FF = D_FFE if moe else D_FF
    ngrp = FF // (128 * G)
    w1s = [B.sb(P, [128, KC, 128 * G], BF16, "w1s") for _ in range(NS)]
    w3s = [B.sb(P, [128, KC, 128 * G], BF16, "w3s") for _ in range(NS)]
    w2s = [B.sb(P, [128, G, D], BF16, "w2s") for _ in range(NS)]
    w1T = [T("w1s%d" % i) for i in range(NS)]
    w3T = [T("w3s%d" % i) for i in range(NS)]
    w2T = [T("w2s%d" % i) for i in range(NS)]
    su = [B.sb(P, [128, 512], BF16, "su") for _ in range(2)]
    suT = [T("su0"), T("su1")]
    actb = [B.sb(P, [128, G, NT], BF16, "actb") for _ in range(2)]
    actT = [[[T("act") for _ in ffn_tiles] for _ in range(G)] for _ in range(2)]
    it = 0
    ub = 0
    yb_i = 0
    for e_i in range(nexp):
        if moe:
            w1d, w3d, w2d = dr["moe_w1"][e_i], dr["moe_w3"][e_i], dr["moe_w2"][e_i]
            ws = e_i % 2
            for hlf in range(2):
                bank = 4
                S.op("pe", lambda e, e_i=e_i, hlf=hlf, bank=bank: e.matmul(ps[bank][:, 0:512], sel[:, e_i * 128:(e_i + 1) * 128], wT8[:, hlf * 512:(hlf + 1) * 512], start=True, stop=True),
                     reads=[selT, wT8T], writes=[psT[bank]])
                B.copy(wb[ws][:, hlf * 512:(hlf + 1) * 512], ps[bank][:, 0:512], [psT[bank]], [wbT[ws]], eng="act")
        else:
            w1d, w3d, w2d = dr["ffn_w1"], dr["ffn_w3"], dr["ffn_w2"]
        for g in range(ngrp):
            s = it % NS
            ab = it % 2
            it += 1
            f0 = g * 128 * G
            B.dma("pool", w1s[s][:], w1d[:, f0:f0 + 128 * G].rearrange("(kc p) f -> p kc f", p=128), [], w1T[s])
            B.dma("pool", w3s[s][:], w3d[:, f0:f0 + 128 * G].rearrange("(kc p) f -> p kc f", p=128), [], w3T[s])
            B.dma("pool", w2s[s][:], w2d[f0:f0 + 128 * G, :].rearrange("(j p) d -> p j d", p=128), [], w2T[s])
            for ti, (kind, c0, n) in enumerate(ffn_tiles):
                t0 = toff[ti]
                for j in range(G):
                    b1 = ub % 2
                    b3 = 2 + (ub % 2)
                    k = ub % 2
                    ub += 1
                    B.mm(b1, ps[b1][:, 0:n], [(w1s[s][:, kc, j * 128:(j + 1) * 128], h2[:, kc, t0:t0 + n]) for kc in range(KC)], [w1T[s], h2T])
                    B.mm(b3, ps[b3][:, 0:n], [(w3s[s][:, kc, j * 128:(j + 1) * 128], h2[:, kc, t0:t0 + n]) for kc in range(KC)], [w3T[s], h2T])
                    B.act(su[k][:, 0:n], ps[b1][:, 0:n], AF.Silu, [psT[b1]], [suT[k]])
                    aT = actT[ab][j][ti]
                    B.tt(actb[ab][:, j, t0:t0 + n], su[k][:, 0:n], ps[b3][:, 0:n], ALU.mult, [suT[k], psT[b3]], [aT])
                    if moe:
                        B.tt(actb[ab][:, j, t0:t0 + n], actb[ab][:, j, t0:t0 + n], wb[ws][:, t0:t0 + n], ALU.mult, [aT, wbT[ws]], [aT])
            for ti, (kind, c0, n) in enumerate(ffn_tiles):
                t0 = toff[ti]
                col = 1 if kind == "ctx" else 0
                xs, xsT = xview(kind, c0, n)
                for dc in range(KC):
                    bank = 5 + (yb_i % 3)
                    yb_i += 1
                    B.mm(bank, ps[bank][:, 0:n], [(w2s[s][:, j, dc * 128:(dc + 1) * 128], actb[ab][:, j, t0:t0 + n]) for j in range(G)],
                         [w2T[s]] + [actT[ab][j][ti] for j in range(G)])
                    B.stt(xs[dc], ps[bank][:, 0:n], mod_ap(5, dc, col), xs[dc], ALU.mult, ALU.add, [psT[bank], modTT] + flat(xsT[dc]), flat(xsT[dc]))


def build_program(lyr, last):
    nc = bass.Bass("TRN2", target_bir_lowering=False)
    moe = (lyr % 2 == 1)
    dr = {}

    def inp(name, shape, dt=F32):
        dr[name] = nc.dram_tensor(name, list(shape), dt, kind="ExternalInput").ap()

    inp("xT", [D, NKS])
    inp("rope", [128, 4, NKS])
    inp("pp", [128, NPP])
    inp("cb", [128, NCB], BF16)
    inp("idf", [128, 128])
    inp("ada_w", [D, 6 * D])
    inp("w_in", [D, 4608])
    inp("w_out", [D, D])
    if moe:
        inp("sel", [8, 1024])
        inp("moe_w1", [NEXP, D, D_FFE])
        inp("moe_w3", [NEXP, D, D_FFE])
        inp("moe_w2", [NEXP, D_FFE, D])
    else:
        inp("ffn_w1", [D, D_FF])
        inp("ffn_w3", [D, D_FF])
        inp("ffn_w2", [D_FF, D])
    dr["kTA"] = nc.dram_tensor("kTA", [4, 128, NKS], BF16).ap()
    dr["kTC"] = nc.dram_tensor("kTC", [2, 128, NKS], BF16).ap()
    dr["vA"] = nc.dram_tensor("vA", [NKS, 512], BF16).ap()
    dr["vC"] = nc.dram_tensor("vC", [NKS, 256], BF16).ap()
    dr["xo"] = nc.dram_tensor("xo", [D, TOWN], F32, kind="ExternalOutput").ap()
    if not last:
        dr["xco"] = nc.dram_tensor("xco", [D, LCTX], F32, kind="ExternalOutput").ap()
    B = Builder(nc)
    outs = build_layer(B, lyr, dr, last)
    B.S.emit(final_waits=outs)
    return nc


def _rope_tables(pos, valid_lat):
    f32 = np.float32
    p = np.where(valid_lat, pos, 0)
    row = (p // 64).astype(f32)
    colp = (p % 64).astype(f32)
    out = np.zeros((128, 4, len(pos)), f32)
    for gi, dh in ((0, 64), (1, 128)):
        half = dh // 2
        inv = (f32(10000.0) ** (-np.arange(0, half, 2, dtype=f32) / f32(half))).astype(f32)
        nf = len(inv)
        ar = (row[:, None] * inv[None, :]).astype(f32)
        ac = (colp[:, None] * inv[None, :]).astype(f32)
        ang = np.concatenate([ar, ar, ac, ac], axis=-1)
        c = np.cos(ang).astype(f32).T
        s = np.sin(ang).astype(f32).T
        c = np.where(valid_lat[None, :], c, 1.0).astype(f32)
        s = np.where(valid_lat[None, :], s, 0.0).astype(f32)
        if dh == 64:
            c = np.concatenate([c, c], 0)
            s = np.concatenate([s, s], 0)
        out[:, 2 * gi] = c
        out[:, 2 * gi + 1] = s
    return out


def _const_pack():
    f32 = np.float32
    cbm = np.zeros((128, NCB), f32)
    cbm[:, CB_ONES:CB_ONES + 128] = 1.0
    i = np.arange(128)
    cbm[:, CB_BLK:CB_BLK + 128] = (i[:, None] // 64 == i[None, :] // 64)
    for off, dh in ((CB_ROTA, 64), (CB_ROTC, 128)):
        q = dh // 4
        M = np.zeros((128, 128), f32)
        for base in range(0, 128, dh):
            for ii in range(dh):
                qq = ii // q
                if qq in (0, 2):
                    M[base + ii + q, base + ii] = -1.0
                else:
                    M[base + ii - q, base + ii] = 1.0
        cbm[:, off:off + 128] = M
    cbm[:, CB_MLO:CB_MLO + 128] = (i[None, :] <= i[:, None])
    cbm[:, CB_MUP:CB_MUP + 128] = (i[:, None] <= i[None, :])
    sel = np.zeros((8, 1024), f32)
    for e in range(8):
        sel[e, e * 128:(e + 1) * 128] = 1.0
    return cbm.astype(ml_dtypes.bfloat16), np.eye(128, dtype=f32), sel


def _key_stream(j):
    s0 = j * TOWN
    src = np.zeros(NKS, np.int64)
    valid = np.zeros(NKS, bool)
    pos = np.zeros(NKS, np.int64)
    src[:LCTX] = np.arange(LCTX)
    valid[:LCTX] = True
    wpos = np.arange(s0 - 128, s0 + TOWN + 128)
    wv = (wpos >= 0) & (wpos < SEQ)
    src[WOFF:WOFF + 1280] = np.where(wv, LCTX + wpos, 0)
    valid[WOFF:WOFF + 1280] = wv
    pos[WOFF:WOFF + 1280] = wpos
    rest = np.concatenate([np.arange(0, max(s0 - 128, 0)), np.arange(min(s0 + TOWN + 128, SEQ), SEQ)])
    nr = len(rest)
    src[1536:1536 + nr] = LCTX + rest
    valid[1536:1536 + nr] = True
    pos[1536:1536 + nr] = rest
    lat = valid.copy()
    lat[:LCTX] = False
    return src, valid, pos, lat


def _pp_pack(l, b, j, valid, P):
    f32 = np.float32
    pp = np.zeros((128, NPP), f32)

    def put(name, arr):
        o, n = PP[name]
        pp[:, o:o + n] = np.asarray(arr, f32).reshape(128, n)

    cT = np.stack([P["c"][b].reshape(KC, 128).T, P["c_ctx"].reshape(KC, 128).T], axis=-1)
    put("cT", cT.reshape(128, 32))
    put("ada_b", P["ada_b"][l].reshape(96, 128).T)
    put("nmix", P["norm_mix_g"][l].reshape(KC, 128).T)
    put("nffn", P["norm_ffn_g"][l].reshape(KC, 128).T)
    put("aqn", np.tile(P["a_q_norm"][l], 2)[:, None])
    put("akn", np.tile(P["a_k_norm"][l], 2)[:, None])
    put("cqn", P["c_q_norm"][l][:, None])
    put("ckn", P["c_k_norm"][l][:, None])
    lamv = np.concatenate([P["a_lam_q1"][l], P["a_lam_k1"][l], P["a_lam_q2"][l], P["a_lam_k2"][l]])
    put("lamv", np.broadcast_to(lamv[None, :], (128, 256)))
    put("subln", P["a_subln_g"][l][:, None])
    put("convw", P["b_conv_w"][l].reshape(3, 4, 128).transpose(2, 1, 0).reshape(128, 12))
    put("boutg", P["b_out_g"][l].reshape(4, 128).T)
    put("sink", np.broadcast_to(P["c_sink"][l][None, :], (128, 8)))
    put("coutg", P["c_out_g"][l].reshape(8, 128).T)
    if l % 2 == 1:
        put("routw", P["router_w"][l // 2].reshape(KC, 128, 8).transpose(1, 0, 2).reshape(128, 128))
    hv = np.array([1.0 if j > 0 else 0.0, 1.0 if j < SEQ // TOWN - 1 else 0.0], f32)
    put("hval", np.broadcast_to(hv[None, :], (128, 2)))
    kb = np.where(valid, 0.0, NEG).astype(f32).reshape(NKB, 128).T
    put("kbias", kb)
    return pp


_PROG = {}


def _get_prog(lyr, last):
    key = (lyr, last)
    if key not in _PROG:
        _PROG[key] = build_program(lyr, last)
    return _PROG[key]


def _run_layer(l, last, xT_b, xcT_b, P):
    nb = len(xT_b)
    nq = SEQ // TOWN
    cbm, idf, sel = _const_pack()
    in_maps = []
    for core in range(nb * nq):
        b, j = divmod(core, nq)
        src, valid, pos, lat = _key_stream(j)
        xcat = np.concatenate([xcT_b[b], xT_b[b]], axis=1)
        xks = np.ascontiguousarray(xcat[:, src])
        xks[:, ~valid] = 0.0
        m = {"xT": xks, "rope": _rope_tables(pos, lat), "pp": _pp_pack(l, b, j, valid, P), "cb": cbm, "idf": idf,
             "ada_w": P["ada_w"][l], "w_in": P["w_in"][l], "w_out": P["w_out"][l]}
        if l % 2 == 1:
            i = l // 2
            m.update({"sel": sel, "moe_w1": P["moe_w1"][i], "moe_w3": P["moe_w3"][i], "moe_w2": P["moe_w2"][i]})
        else:
            i = l // 2
            m.update({"ffn_w1": P["ffn_w1"][i], "ffn_w3": P["ffn_w3"][i], "ffn_w2": P["ffn_w2"][i]})
        in_maps.append(m)
    nc = _get_prog(l, last)
    res = run_bass_kernel_spmd(nc, in_maps, core_ids=list(range(nb * nq)))
    xo = [np.concatenate([res.results[b * nq + j]["xo"] for j in range(nq)], axis=1) for b in range(nb)]
    xco = None if last else [res.results[b * nq]["xco"] for b in range(nb)]
    return xo, xco


def kernel(**inputs):
    P = {k: np.asarray(v) for k, v in inputs.items()}
    x, ctx = P["x"], P["ctx"]
    nb = x.shape[0]
    xT = [np.ascontiguousarray(x[b].T) for b in range(nb)]
    xcT = [np.ascontiguousarray(ctx[b].T) for b in range(nb)]
    depth = P["w_in"].shape[0]
    for l in range(depth):
        last = l == depth - 1
        xT, xcT_new = _run_layer(l, last, xT, xcT, P)
        if not last:
            xcT = xcT_new
    out = np.stack([np.ascontiguousarray(xT[b].T) for b in range(nb)], axis=0)
    return out.astype(np.float32)
```
